# Optimizing a Trainium2 kernel written in Bass

```python
import jax, jax.numpy as jnp
from jax import lax
import numpy as np

D_MODEL = 2048
BATCH = 4
SEQ = 4096
DEPTH = 1

CHUNK = 64
Q_BLOCK = 2 * CHUNK
MIX_WIDTH = D_MODEL
ATTN_WIDTH = MIX_WIDTH // 2
ATTN_HEADS = 8
ATTN_HEAD_DIM = ATTN_WIDTH // ATTN_HEADS
LRU_WIDTH = MIX_WIDTH - ATTN_WIDTH
LRU_BLOCKS = 8
LRU_BLOCK_DIM = LRU_WIDTH // LRU_BLOCKS
CONV_WIDTH = 4
LRU_C = 8.0
OFF_Q = 0
OFF_K = OFF_Q + ATTN_WIDTH
OFF_V = OFF_K + ATTN_WIDTH
OFF_F = OFF_V + ATTN_WIDTH
OFF_LX = OFF_F + ATTN_HEADS
OFF_LY = OFF_LX + LRU_WIDTH
IN_COLS = OFF_LY + LRU_WIDTH
PEER_HEADS = 8
PEER_N_KEYS = 128
PEER_N_EXPERTS = PEER_N_KEYS * PEER_N_KEYS
PEER_QUERY_DIM = 256
PEER_HALF = PEER_QUERY_DIM // 2
PEER_TOPK = 16
PEER_TOKEN_BLOCK = 128
N_ADA = 6
EPS = 1e-6

kernel_name = 'hymba_fox_rglru_peer_adaln'


def _rmsnorm(x, g):
    xf = x.astype(jnp.float32)
    y = xf * lax.rsqrt(jnp.mean(xf * xf, axis=-1, keepdims=True) + EPS)
    return y.astype(x.dtype) * g


def _forgetting_attention(q, k, v, log_f):
    n_blocks = q.shape[2] // Q_BLOCK
    cum = jnp.cumsum(log_f, axis=-1)
    scale = ATTN_HEAD_DIM ** -0.5
    outs = []
    for blk in range(n_blocks):
        start = blk * Q_BLOCK
        end = start + Q_BLOCK
        qb = q[:, :, start:end]
        kb = k[:, :, :end]
        vb = v[:, :, :end]
        logits = jnp.einsum('bhqd,bhkd->bhqk', qb, kb).astype(jnp.float32) * scale
        logits = logits + cum[:, :, start:end, None] - cum[:, :, None, :end]
        mask = jnp.arange(end)[None, :] <= (start + jnp.arange(Q_BLOCK))[:, None]
        logits = jnp.where(mask, logits, -jnp.inf)
        p = jax.nn.softmax(logits, axis=-1)
        outs.append(jnp.einsum('bhqk,bhkd->bhqd', p.astype(v.dtype), vb))
    return jnp.concatenate(outs, axis=2)


def _causal_depthwise_conv(x, w, b):
    y = lax.conv_general_dilated(
        x, w[:, None, :], window_strides=(1,), padding=[(CONV_WIDTH - 1, 0)],
        dimension_numbers=('NWC', 'WIO', 'NWC'), feature_group_count=x.shape[-1])
    return y + b


def _rg_lru(xc, w_a, b_a, w_x, b_x, lam):
    B, S, _ = xc.shape
    xb = xc.reshape(B, S, LRU_BLOCKS, LRU_BLOCK_DIM)
    r = jax.nn.sigmoid(jnp.einsum('bsni,nij->bsnj', xb, w_a).reshape(B, S, LRU_WIDTH) + b_a)
    i = jax.nn.sigmoid(jnp.einsum('bsni,nij->bsnj', xb, w_x).reshape(B, S, LRU_WIDTH) + b_x)
    log_a = -LRU_C * r.astype(jnp.float32) * jax.nn.softplus(-lam.astype(jnp.float32))
    a = jnp.exp(log_a)
    bterm = jnp.sqrt(-jnp.expm1(2.0 * log_a)) * (i * xc).astype(jnp.float32)

    def combine(lhs, rhs):
        a1, b1 = lhs
        a2, b2 = rhs
        return a1 * a2, a2 * b1 + b2

    _, h = lax.associative_scan(combine, (a, bterm), axis=1)
    return h.astype(xc.dtype)


def _hybrid_mixer(h, w_in, b_f, conv_w, conv_b, lru_w_a, lru_b_a, lru_w_x, lru_b_x,
                  lru_lambda, g_attn_out, g_lru_out, w_out):
    B, S, _ = h.shape
    proj = h @ w_in

    def heads(t):
        return t.reshape(B, S, ATTN_HEADS, ATTN_HEAD_DIM).transpose(0, 2, 1, 3)

    q = heads(proj[..., OFF_Q:OFF_K])
    k = heads(proj[..., OFF_K:OFF_V])
    v = heads(proj[..., OFF_V:OFF_F])
    f_logit = (proj[..., OFF_F:OFF_LX] + b_f).astype(jnp.float32)
    log_f = jax.nn.log_sigmoid(f_logit).transpose(0, 2, 1)
    attn = _forgetting_attention(q, k, v, log_f)
    attn = attn.transpose(0, 2, 1, 3).reshape(B, S, ATTN_WIDTH)

    lx = proj[..., OFF_LX:OFF_LY]
    ly = proj[..., OFF_LY:IN_COLS]
    xc = _causal_depthwise_conv(lx, conv_w, conv_b)
    lru = _rg_lru(xc, lru_w_a, lru_b_a, lru_w_x, lru_b_x, lru_lambda) * jax.nn.gelu(ly)

    merged = jnp.concatenate([_rmsnorm(attn, g_attn_out), _rmsnorm(lru, g_lru_out)], axis=-1)
    return merged @ w_out


def _peer(h, w_q, k1, k2, u, vt):
    B, S, D = h.shape
    q = (h @ w_q).reshape(B, S, PEER_HEADS, 2, PEER_HALF)
    s1 = jnp.einsum('bshd,hnd->bshn', q[..., 0, :], k1).astype(jnp.float32)
    s2 = jnp.einsum('bshd,hnd->bshn', q[..., 1, :], k2).astype(jnp.float32)
    v1, i1 = lax.top_k(s1, PEER_TOPK)
    v2, i2 = lax.top_k(s2, PEER_TOPK)
    cand = (v1[..., :, None] + v2[..., None, :]).reshape(B, S, PEER_HEADS, PEER_TOPK * PEER_TOPK)
    vs, ic = lax.top_k(cand, PEER_TOPK)
    e = (jnp.take_along_axis(i1, ic // PEER_TOPK, axis=-1) * PEER_N_KEYS
         + jnp.take_along_axis(i2, ic % PEER_TOPK, axis=-1))
    w = jax.nn.softmax(vs, axis=-1).astype(h.dtype)

    nb = (B * S) // PEER_TOKEN_BLOCK
    hb = h.reshape(nb, PEER_TOKEN_BLOCK, D)
    eb = e.reshape(nb, PEER_TOKEN_BLOCK, PEER_HEADS, PEER_TOPK)
    wb = w.reshape(nb, PEER_TOKEN_BLOCK, PEER_HEADS, PEER_TOPK)

    def expert_block(args):
        hx, ex, wx = args
        ug = jnp.take(u, ex, axis=0)
        act = jax.nn.gelu(jnp.einsum('thkd,td->thk', ug, hx)) * wx
        vg = jnp.take(vt, ex, axis=0)
        return jnp.einsum('thk,thkd->td', act, vg)

    y = lax.map(expert_block, (hb, eb, wb))
    return y.reshape(B, S, D)


def setup_inputs(seed: int = 0) -> dict:
    key = jax.random.key(seed)
    ks = jax.random.split(key, 26)
    f32 = jnp.float32
    L, D = DEPTH, D_MODEL

    def nrm(k, shape, scale):
        return jax.random.normal(k, shape, f32) * scale

    a0 = jax.random.uniform(ks[12], (L, LRU_WIDTH), f32, 0.9, 0.999) ** (1.0 / LRU_C)
    return {
        'x': nrm(ks[0], (BATCH, SEQ, D), 1.0),
        'c': nrm(ks[1], (BATCH, D), 1.0),
        'w_ada': nrm(ks[2], (L, D, N_ADA * D), 0.5 * D ** -0.5),
        'b_ada': nrm(ks[3], (L, N_ADA * D), 0.01),
        'g_mix': 1.0 + nrm(ks[4], (L, D), 0.05),
        'w_in': nrm(ks[5], (L, D, IN_COLS), D ** -0.5),
        'b_f': jax.random.uniform(ks[6], (L, ATTN_HEADS), f32, 1.0, 4.0),
        'conv_w': nrm(ks[7], (L, CONV_WIDTH, LRU_WIDTH), CONV_WIDTH ** -0.5),
        'conv_b': nrm(ks[8], (L, LRU_WIDTH), 0.01),
        'lru_w_a': nrm(ks[9], (L, LRU_BLOCKS, LRU_BLOCK_DIM, LRU_BLOCK_DIM), LRU_BLOCK_DIM ** -0.5),
        'lru_b_a': nrm(ks[10], (L, LRU_WIDTH), 0.01),
        'lru_w_x': nrm(ks[11], (L, LRU_BLOCKS, LRU_BLOCK_DIM, LRU_BLOCK_DIM), LRU_BLOCK_DIM ** -0.5),
        'lru_b_x': nrm(ks[13], (L, LRU_WIDTH), 0.01),
        'lru_lambda': jnp.log(a0) - jnp.log1p(-a0),
        'g_attn_out': 1.0 + nrm(ks[14], (L, ATTN_WIDTH), 0.05),
        'g_lru_out': 1.0 + nrm(ks[15], (L, LRU_WIDTH), 0.05),
        'w_out': nrm(ks[16], (L, MIX_WIDTH, D), MIX_WIDTH ** -0.5),
        'g_ffn': 1.0 + nrm(ks[17], (L, D), 0.05),
        'peer_w_q': nrm(ks[18], (L, D, PEER_HEADS * PEER_QUERY_DIM), D ** -0.5),
        'peer_k1': nrm(ks[19], (L, PEER_HEADS, PEER_N_KEYS, PEER_HALF), PEER_HALF ** -0.5),
        'peer_k2': nrm(ks[20], (L, PEER_HEADS, PEER_N_KEYS, PEER_HALF), PEER_HALF ** -0.5),
        'peer_u': nrm(ks[21], (L, PEER_N_EXPERTS, D), D ** -0.5),
        'peer_v': nrm(ks[22], (L, PEER_N_EXPERTS, D), 1.0),
        'g_final': 1.0 + nrm(ks[23], (D,), 0.05),
    }


def reference(x, c, w_ada, b_ada, g_mix, w_in, b_f, conv_w, conv_b, lru_w_a, lru_b_a,
              lru_w_x, lru_b_x, lru_lambda, g_attn_out, g_lru_out, w_out, g_ffn,
              peer_w_q, peer_k1, peer_k2, peer_u, peer_v, g_final):
    sc = jax.nn.silu(c)
    for l in range(DEPTH):
        mod = sc @ w_ada[l] + b_ada[l]
        sh1, sc1, ga1, sh2, sc2, ga2 = jnp.split(mod, N_ADA, axis=-1)
        h = _rmsnorm(x, g_mix[l]) * (1.0 + sc1[:, None, :]) + sh1[:, None, :]
        mix = _hybrid_mixer(h, w_in[l], b_f[l], conv_w[l], conv_b[l], lru_w_a[l], lru_b_a[l],
                            lru_w_x[l], lru_b_x[l], lru_lambda[l], g_attn_out[l],
                            g_lru_out[l], w_out[l])
        x = x + ga1[:, None, :] * mix
        h = _rmsnorm(x, g_ffn[l]) * (1.0 + sc2[:, None, :]) + sh2[:, None, :]
        x = x + ga2[:, None, :] * _peer(h, peer_w_q[l], peer_k1[l], peer_k2[l], peer_u[l], peer_v[l])
    return _rmsnorm(x, g_final)
```

```python
from contextlib import ExitStack
import numpy as np
import concourse.bass as bass
import concourse.mybir as mybir
from concourse.bass_utils import run_bass_kernel_spmd

F32 = mybir.dt.float32
BF16 = mybir.dt.bfloat16
AF = mybir.ActivationFunctionType
ALU = mybir.AluOpType

D = 2048
DC = 16
TB = 512
H = 8
NEXP = 16384
EPS = 1e-6
NEG = -30000.0
BIGM = 1.0e6
OFF_Q, OFF_K, OFF_V, OFF_F, OFF_LX, OFF_LY, IN_COLS = 0, 1024, 2048, 3072, 3080, 4104, 5128
GE = 512
NG = NEXP // GE


class Buf:
    __slots__ = ("w", "r")

    def __init__(self):
        self.w = None
        self.r = {}


def bufs(n):
    return [Buf() for _ in range(n)]


class Sched:
    ENG = ("pe", "act", "dve", "pool", "sp")

    def __init__(self, nc, st, n_dma_sems=48):
        self.nc = nc
        self.streams = {e: [] for e in self.ENG}
        self.cnt = {e: 0 for e in self.ENG}
        self.waited = {e: {} for e in self.ENG}
        self.n_dma = n_dma_sems
        self.dma_next = 0
        self.dma_val = [0] * n_dma_sems
        self.sems = {}
        for e in self.ENG:
            self.sems[e] = st.enter_context(nc.semaphore("s_" + e))
        for k in range(n_dma_sems):
            self.sems[("dma", k)] = st.enter_context(nc.semaphore("s_dma%d" % k))

    def _deps(self, eng, reads, writes):
        need = {}

        def add(s, v):
            if need.get(s, 0) < v:
                need[s] = v

        for b in reads:
            if b.w is not None:
                add(*b.w)
        for b in writes:
            if b.w is not None:
                add(*b.w)
            for s, v in b.r.items():
                add(s, v)
        waits = []
        for s, v in need.items():
            if s == eng and eng == "pe":
                continue
            if self.waited[eng].get(s, 0) >= v:
                continue
            self.waited[eng][s] = v
            waits.append((s, v))
        return waits

    def _mark(self, ev, reads, writes):
        s, v = ev
        for b in reads:
            if b.r.get(s, 0) < v:
                b.r[s] = v
        for b in writes:
            b.w = ev
            b.r = {}

    def op(self, eng, fn, reads=(), writes=()):
        waits = self._deps(eng, reads, writes)
        self.cnt[eng] += 1
        ev = (eng, self.cnt[eng])
        self.streams[eng].append((waits, fn, (eng, 1)))
        self._mark(ev, reads, writes)

    def dma(self, eng, fn, reads=(), writes=()):
        k = self.dma_next
        self.dma_next = (k + 1) % self.n_dma
        s = ("dma", k)
        waits = self._deps(eng, reads, writes)
        if self.dma_val[k] > 0 and self.waited[eng].get(s, 0) < self.dma_val[k]:
            waits.append((s, self.dma_val[k]))
            self.waited[eng][s] = self.dma_val[k]
        self.dma_val[k] += 16
        ev = (s, self.dma_val[k])
        self.streams[eng].append((waits, fn, (s, 16)))
        self._mark(ev, reads, writes)

    def emit_phase(self):
        nc = self.nc
        final = []
        for k in range(self.n_dma):
            if self.dma_val[k] > 0:
                final.append((("dma", k), self.dma_val[k]))
        for e in ("pe", "act", "dve", "pool"):
            if self.cnt[e] > 0:
                final.append((e, self.cnt[e]))
        streams = self.streams
        self.streams = {e: [] for e in self.ENG}
        sems = self.sems
        waited = self.waited

        def run(engh, name):
            for waits, fn, inc in streams[name]:
                for s, v in waits:
                    engh.wait_ge(sems[s], v)
                ins = fn(engh)
                ins.then_inc(sems[inc[0]], inc[1])
            for s, v in final:
                if s == name:
                    continue
                if waited[name].get(s, 0) < v:
                    engh.wait_ge(sems[s], v)
                    waited[name][s] = v

        with nc.Block() as block:
            @block.tensor
            def _(e):
                run(e, "pe")

            @block.scalar
            def _(e):
                run(e, "act")

            @block.vector
            def _(e):
                run(e, "dve")

            @block.gpsimd
            def _(e):
                run(e, "pool")

            @block.sync
            def _(e):
                run(e, "sp")


def build_nc(NC, NO, stop_after=99, debug=()):
    nc = bass.Bass("TRN2", target_bir_lowering=False)
    TC, TO = NC * TB, NO * TB
    TT = TC + TO
    NKT = TT // 128

    def din(name, shape, dt=F32):
        return nc.dram_tensor(name, list(shape), dt, kind="ExternalInput").ap()

    xc_d = din("xc", [TC, D])
    xo_d = din("xo", [TO, D])
    ctxb_d = din("ctxb", [128, 1])
    ctxk_d = din("ctxk", [128, 1])
    ccol_d = din("c_col", [128, DC])
    wada_d = din("w_ada", [D, 6 * D])
    bada_d = din("b_ada", [1, 6 * D])
    gmix_d = din("g_mix_col", [128, DC])
    gffn_d = din("g_ffn_col", [128, DC])
    win_d = din("w_in", [D, IN_COLS])
    nbf_d = din("nb_f_col", [H, 1])
    convw_d = din("conv_w_col", [128, 8, 4])
    convb_d = din("conv_b_col", [128, 8])
    lba_d = din("lru_b_a_col", [128, 8])
    lbx_d = din("lru_b_x_col", [128, 8])
    lam_d = din("lru_lam_col", [128, 8])
    lwa_d = din("lru_w_a", [128, 8, 128])
    lwx_d = din("lru_w_x", [128, 8, 128])
    gmrg_d = din("g_mrg_col", [128, DC])
    wout_d = din("w_out", [D, D])
    wq_d = din("peer_w_q", [D, D])
    k1t_d = din("k1t", [128, H, 128])
    k2t_d = din("k2t", [128, H, 128])
    ut_d = din("peer_ut", [D, NEXP])
    vt_d = din("peer_v", [NEXP, D])
    gfin_d = din("g_final", [1, D])
    onehot_d = din("onehot", [96, H, 128])
    out_d = nc.dram_tensor("out", [TO, D], F32, kind="ExternalOutput").ap()

    def dscr(name, shape, dt):
        return nc.dram_tensor(name, list(shape), dt).ap()

    KT_d = dscr("KT_s", [H, 128, TT], BF16); B_KT = Buf()
    V_d = dscr("V_s", [H, TT, 128], BF16); B_V = Buf()
    QT_d = dscr("QT_s", [H, 128, TO], BF16); B_QT = Buf()
    NCUM_d = dscr("NCUM_s", [H, TT], F32); B_NCUM = Buf()
    LRUT_d = dscr("LRUT_s", [8, 128, TO], BF16); B_LRUT = Buf()
    X1_d = dscr("X1_s", [TO, D], F32); B_X1 = Buf()
    H2T_d = dscr("H2T_s", [128, DC, TO], BF16); B_H2T = Buf()
    GAR_d = dscr("GAR_s", [2, D], F32); B_GAR = bufs(2)

    dbg = {}

    def dbg_out(name, shape, dt=F32):
        dbg[name] = nc.dram_tensor("dbg_" + name, list(shape), dt, kind="ExternalOutput").ap()
        return dbg[name]

    with ExitStack() as gst:
        S = Sched(nc, gst)

        uid = [0]

        def sbt(st, name, shape, dt):
            uid[0] += 1
            return st.enter_context(nc.sbuf_tensor("%s_%d" % (name, uid[0]), list(shape), dt))

        def pst(st, name, shape, dt):
            uid[0] += 1
            return st.enter_context(nc.psum_tensor("%s_%d" % (name, uid[0]), list(shape), dt))

        def DMA(eng, out, in_, reads=(), writes=(), slow=False):
            if slow:
                S.dma(eng, lambda e: e.dma_start(out=out, in_=in_, allow_slow_non_contiguous=True),
                      reads=reads, writes=writes)
            else:
                S.dma(eng, lambda e: e.dma_start(out=out, in_=in_), reads=reads, writes=writes)

        def MM(out, lhsT, rhs, start, stop, reads=(), writes=()):
            S.op("pe", lambda e: e.matmul(out, lhsT=lhsT, rhs=rhs, start=start, stop=stop), reads=reads, writes=writes)

        def TR(out, in_, ident, reads=(), writes=()):
            S.op("pe", lambda e: e.matmul(out, lhsT=in_, rhs=ident, start=True, stop=True), reads=reads, writes=writes)

        def ACT(out, in_, func, reads=(), writes=(), bias=None, scale=None, accum=None):
            kw = {}
            if bias is not None:
                kw["bias"] = bias
            if scale is not None:
                kw["scale"] = scale
            if accum is not None:
                kw["accum_out"] = accum
            S.op("act", lambda e: e.activation(out=out, in_=in_, func=func, **kw), reads=reads, writes=writes)

        def TS(eng, out, in0, s1, s2, op0, op1=None, reads=(), writes=()):
            if op1 is None:
                S.op(eng, lambda e: e.tensor_scalar(out=out, in0=in0, scalar1=s1, scalar2=None, op0=op0),
                     reads=reads, writes=writes)
            else:
                S.op(eng, lambda e: e.tensor_scalar(out=out, in0=in0, scalar1=s1, scalar2=s2, op0=op0, op1=op1),
                     reads=reads, writes=writes)

        def STT(eng, out, in0, scalar, in1, op0, op1, reads=(), writes=()):
            S.op(eng, lambda e: e.scalar_tensor_tensor(out=out, in0=in0, scalar=scalar, in1=in1, op0=op0, op1=op1),
                 reads=reads, writes=writes)

        def TTE(eng, out, in0, in1, op, reads=(), writes=()):
            S.op(eng, lambda e: e.tensor_tensor(out=out, in0=in0, in1=in1, op=op), reads=reads, writes=writes)

        def CP(eng, out, in_, reads=(), writes=()):
            if eng == "act":
                S.op("act", lambda e: e.copy(out=out, in_=in_), reads=reads, writes=writes)
            else:
                S.op(eng, lambda e: e.tensor_copy(out=out, in_=in_), reads=reads, writes=writes)

        def MEMSET(eng, ap, val, writes=()):
            S.op(eng, lambda e: e.memset(ap, val), writes=writes)

        def RSTD(ss, rstd, n, B_ss, B_rstd):
            ACT(rstd, ss, AF.Sqrt, reads=[B_ss], writes=[B_rstd], scale=1.0 / n, bias=EPS)
            S.op("dve", lambda e: e.reciprocal(out=rstd, in_=rstd), reads=[B_rstd], writes=[B_rstd])

        ident_bf = sbt(gst, "ident_bf", [128, 128], BF16); B_ident = Buf()
        ident_f = sbt(gst, "ident_f", [128, 128], F32); B_identf = Buf()
        ones_row = sbt(gst, "ones_row", [1, 128], F32); B_ones = Buf()
        dummy = sbt(gst, "dummy", [1, 8], F32); B_dummy = Buf()
        modT = sbt(gst, "modT", [128, 96], F32); B_modT = Buf()
        A1 = sbt(gst, "A1", [128, DC], F32); B_A1 = Buf()
        A2 = sbt(gst, "A2", [128, DC], F32); B_A2 = Buf()
        GA2b = sbt(gst, "GA2b", [128, D], F32); B_GA2b = Buf()
        rs_l = sbt(gst, "rs_l", [128, NO * 4], F32); B_rsl = Buf()
        ctxb = sbt(gst, "ctxb_t", [128, 1], F32); B_ctxb = Buf()
        ctxk = sbt(gst, "ctxk_t", [128, 1], F32); B_ctxk = Buf()
        mid = gst.enter_context(ExitStack())
        GA1b = sbt(mid, "GA1b", [128, D], F32); B_GA1b = Buf()

        S.op("pool", lambda e: e.memset(ident_f[:], 0.0), writes=[B_identf])
        S.op("pool", lambda e: e.affine_select(out=ident_f[:], in_=ident_f[:], pattern=[[-1, 128]],
                                               compare_op=ALU.not_equal, fill=1.0, base=0,
                                               channel_multiplier=1), reads=[B_identf], writes=[B_identf])
        CP("pool", ident_bf[:], ident_f[:], reads=[B_identf], writes=[B_ident])
        MEMSET("pool", ones_row[:], 1.0, writes=[B_ones])
        DMA("sp", ctxb[:], ctxb_d[:, :], writes=[B_ctxb])
        DMA("sp", ctxk[:], ctxk_d[:, :], writes=[B_ctxk])

        with ExitStack() as st:
            ccol = sbt(st, "ccol", [128, DC], F32); B_cc = Buf()
            scb = sbt(st, "scb", [128, DC], BF16); B_scb = Buf()
            gm = sbt(st, "gm", [128, DC], F32); B_gm = Buf()
            gf = sbt(st, "gf", [128, DC], F32); B_gf = Buf()
            modrow = sbt(st, "modrow", [1, 2048], F32); B_mr = Buf()
            brow = sbt(st, "brow", [1, 2048], F32); B_br = Buf()
            wa = [sbt(st, "wa%d" % i, [128, 2048], BF16) for i in range(3)]; B_wa = bufs(3)
            pb = [pst(st, "pb%d" % i, [128, 512], F32) for i in range(8)]; B_pb = bufs(8)
            DMA("sp", ccol[:], ccol_d[:, :], writes=[B_cc])
            DMA("sp", gm[:], gmix_d[:, :], writes=[B_gm])
            DMA("sp", gf[:], gffn_d[:, :], writes=[B_gf])
            ACT(scb[:], ccol[:], AF.Silu, reads=[B_cc], writes=[B_scb])
            pcol = pb[7]; B_pcol = B_pb[7]
            li = 0
            for r in range(6):
                for kc in range(DC):
                    w = wa[li % 3]; bw = B_wa[li % 3]; li += 1
                    DMA("pool", w[:], wada_d[kc * 128:(kc + 1) * 128, r * 2048:(r + 1) * 2048], writes=[bw])
                    for n in range(4):
                        MM(pb[n][0:1, :], scb[:, kc:kc + 1], w[:, n * 512:(n + 1) * 512], kc == 0, kc == DC - 1,
                           reads=[bw, B_scb], writes=[B_pb[n]])
                DMA("sp", brow[:], bada_d[0:1, r * 2048:(r + 1) * 2048], writes=[B_br])
                for n in range(4):
                    TTE("dve", modrow[0:1, n * 512:(n + 1) * 512], pb[n][0:1, :], brow[0:1, n * 512:(n + 1) * 512],
                        ALU.add, reads=[B_pb[n], B_br], writes=[B_mr])
                for jj in range(16):
                    j = r * 16 + jj
                    MM(pcol[:, j:j + 1], modrow[0:1, jj * 128:(jj + 1) * 128], ones_row[0:1, 0:1], True, True,
                       reads=[B_mr, B_ones], writes=[B_pcol])
                if r in (2, 5):
                    dst, bdst = (GA1b, B_GA1b) if r == 2 else (GA2b, B_GA2b)
                    gi_ = 0 if r == 2 else 1
                    DMA("sp", GAR_d[gi_:gi_ + 1, :], modrow[0:1, :], reads=[B_mr], writes=[B_GAR[gi_]])
                    DMA("sp", dst[:], GAR_d[gi_:gi_ + 1, :].partition_broadcast(128), reads=[B_GAR[gi_]],
                        writes=[bdst])
            CP("dve", modT[:], pcol[:, 0:96], reads=[B_pcol], writes=[B_modT])
            STT("dve", A1[:], modT[:, 16:32], 1.0, gm[:], ALU.add, ALU.mult, reads=[B_modT, B_gm], writes=[B_A1])
            STT("dve", A2[:], modT[:, 64:80], 1.0, gf[:], ALU.add, ALU.mult, reads=[B_modT, B_gf], writes=[B_A2])
            if "mod" in debug:
                d = dbg_out("modT", [128, 96]); DMA("sp", d[:, :], modT[:], reads=[B_modT])
                d = dbg_out("A1", [128, DC]); DMA("sp", d[:, :], A1[:], reads=[B_A1])
                d = dbg_out("GA1b", [128, D]); DMA("sp", d[:, :], GA1b[:], reads=[B_GA1b])
                d = dbg_out("GA2b", [128, D]); DMA("sp", d[:, :], GA2b[:], reads=[B_GA2b])
            S.emit_phase()
        SH1 = modT[:, 0:16]
        SH2 = modT[:, 48:64]
        if stop_after <= 0:
            return nc, dbg

        with ExitStack() as st:
            xt = [sbt(st, "xt%d" % i, [128, D], F32) for i in range(2)]; B_xt = bufs(2)
            xs = [sbt(st, "xs%d" % i, [128, D], BF16) for i in range(4)]; B_xs = bufs(4)
            junk = sbt(st, "junk", [128, D], BF16); B_junk = Buf()
            ss = [sbt(st, "ss%d" % i, [128, 1], F32) for i in range(2)]; B_ss = bufs(2)
            rstd = [sbt(st, "rstd%d" % i, [128, 1], F32) for i in range(2)]; B_rstd = bufs(2)
            hT = [sbt(st, "hT%d" % i, [128, DC, TB], BF16) for i in range(2)]; B_hT = bufs(2)
            wb = [sbt(st, "wb%d" % i, [128, DC, 512], BF16) for i in range(3)]; B_wb = bufs(3)
            wf = sbt(st, "wf", [128, DC, H], BF16); B_wf = Buf()
            ev = [sbt(st, "ev%d" % i, [128, 512], BF16) for i in range(3)]; B_ev = bufs(3)
            nbf = sbt(st, "nbf", [H, 1], F32); B_nbf = Buf()
            fe = sbt(st, "fe", [H, TB], F32); B_fe = Buf()
            fl = sbt(st, "fl", [H, TB], F32); B_fl = Buf()
            fones = sbt(st, "fones", [H, TB], F32); B_fones = Buf()
            ncum = [sbt(st, "ncum%d" % i, [H, TB], F32) for i in range(2)]; B_ncum = bufs(2)
            convw = sbt(st, "convw", [128, 8, 4], F32); B_convw = Buf()
            convb = sbt(st, "convb", [128, 8], F32); B_convb = Buf()
            lba = sbt(st, "lba", [128, 8], F32); B_lba = Buf()
            lbx = sbt(st, "lbx", [128, 8], F32); B_lbx = Buf()
            lam = sbt(st, "lam", [128, 8], F32); B_lam = Buf()
            sca = sbt(st, "sca", [128, 8], F32); B_sca = Buf()
            sca2 = sbt(st, "sca2", [128, 8], F32); B_sca2 = Buf()
            ltmp = sbt(st, "ltmp", [128, 8], F32); B_ltmp = Buf()
            lwa = sbt(st, "lwa", [128, 8, 128], BF16); B_lwa = Buf()
            lwx = sbt(st, "lwx", [128, 8, 128], BF16); B_lwx = Buf()
            lxb = [sbt(st, "lxb%d" % n, [128, 3 + TB], F32) for n in range(8)]; B_lxb = bufs(8)
            carry = sbt(st, "carry", [128, 8], F32); B_carry = bufs(8)
            xcv = [sbt(st, "xcv%d" % i, [128, TB], F32) for i in range(2)]; B_xcv = bufs(2)
            xcb = [sbt(st, "xcb%d" % i, [128, TB], BF16) for i in range(2)]; B_xcb = bufs(2)
            gr = [sbt(st, "gr%d" % i, [128, TB], F32) for i in range(2)]; B_gr = bufs(2)
            gi = [sbt(st, "gi%d" % i, [128, TB], F32) for i in range(2)]; B_gi = bufs(2)
            ga_ = [sbt(st, "ga_%d" % i, [128, TB], F32) for i in range(2)]; B_ga = bufs(2)
            gq = [sbt(st, "gq%d" % i, [128, TB], F32) for i in range(2)]; B_gq = bufs(2)
            hh = [sbt(st, "hh%d" % i, [128, TB], F32) for i in range(2)]; B_hh = bufs(2)
            gly = sbt(st, "gly", [128, 8, TB], BF16); B_gly = bufs(8)
            lsq = [sbt(st, "lsq%d" % i, [128, TB], BF16) for i in range(2)]; B_lsq = bufs(2)
            ones_bf = sbt(st, "ones_bf", [128, 1], BF16); B_onesbf = Buf()
            ssl = sbt(st, "ssl", [128, 4], F32); B_ssl = Buf()
            ssrow = sbt(st, "ssrow", [1, TB], F32); B_ssrow = Buf()
            ptr = pst(st, "ptr", [128, 2, TB], F32); B_ptr = bufs(2)
            pp = [pst(st, "pp%d" % i, [128, 512], F32) for i in range(2)]; B_pp = bufs(2)
            pg = [pst(st, "pg%d" % i, [128, 512], F32) for i in range(2)]; B_pg = bufs(2)
            pf = pst(st, "pf", [128, 512], F32); B_pf = Buf()
            pss = pst(st, "pss", [128, 512], F32); B_pss = Buf()

            DMA("sp", nbf[:], nbf_d[:, :], writes=[B_nbf])
            DMA("sp", convw[:], convw_d[:, :, :], writes=[B_convw])
            DMA("sp", convb[:], convb_d[:, :], writes=[B_convb])
            DMA("sp", lba[:], lba_d[:, :], writes=[B_lba])
            DMA("sp", lbx[:], lbx_d[:, :], writes=[B_lbx])
            DMA("sp", lam[:], lam_d[:, :], writes=[B_lam])
            DMA("pool", lwa[:], lwa_d[:, :, :], writes=[B_lwa])
            DMA("pool", lwx[:], lwx_d[:, :, :], writes=[B_lwx])
            DMA("pool", wf[:], win_d[:, OFF_F:OFF_F + H].rearrange("(dc p) c -> p dc c", p=128), writes=[B_wf],
                slow=True)
            MEMSET("pool", fones[:], 1.0, writes=[B_fones])
            MEMSET("pool", ones_bf[:], 1.0, writes=[B_onesbf])
            MEMSET("pool", carry[:], 0.0, writes=B_carry)
            for n in range(8):
                MEMSET("pool", lxb[n][:, 0:3], 0.0, writes=[B_lxb[n]])
            ACT(ltmp[:], lam[:], AF.Exp, reads=[B_lam], writes=[B_ltmp], scale=-1.0)
            TS("dve", sca[:], ltmp[:], 1.0 / 3.0, -0.5, ALU.mult, ALU.add, reads=[B_ltmp], writes=[B_sca])
            TTE("dve", sca[:], sca[:], ltmp[:], ALU.mult, reads=[B_sca, B_ltmp], writes=[B_sca])
            TS("dve", sca[:], sca[:], 1.0, None, ALU.add, reads=[B_sca], writes=[B_sca])
            TTE("dve", sca[:], sca[:], ltmp[:], ALU.mult, reads=[B_sca, B_ltmp], writes=[B_sca])
            TS("dve", sca2[:], sca[:], -16.0, None, ALU.mult, reads=[B_sca], writes=[B_sca2])
            TS("dve", sca[:], sca[:], -8.0, None, ALU.mult, reads=[B_sca, B_sca2], writes=[B_sca])
            fcarry = None
            wli = 0
            evi = 0
            ppi = 0

            def load_w(c0):
                nonlocal wli
                w = wb[wli % 3]; bw = B_wb[wli % 3]; wli += 1
                DMA("pool", w[:], win_d[:, c0:c0 + 512].rearrange("(dc p) c -> p dc c", p=128), writes=[bw])
                return w, bw

            for blk in range(0 if "skip_blocks" in debug else NC + NO):
                own = blk >= NC
                ob = blk - NC
                xsrc = xo_d[ob * TB:(ob + 1) * TB, :] if own else xc_d[blk * TB:(blk + 1) * TB, :]
                h = hT[blk % 2]; bh = B_hT[blk % 2]
                for tt in range(4):
                    x = xt[tt % 2]; bx = B_xt[tt % 2]
                    s_ = ss[tt % 2]; bs = B_ss[tt % 2]
                    r_ = rstd[tt % 2]; br_ = B_rstd[tt % 2]
                    DMA("sp", x[:], xsrc[tt * 128:(tt + 1) * 128, :], writes=[bx])
                    ACT(junk[:], x[:], AF.Square, reads=[bx], writes=[B_junk, bs], accum=s_[:])
                    RSTD(s_[:], r_[:], D, bs, br_)
                    ACT(xs[tt][:], x[:], AF.Copy, reads=[bx, br_], writes=[B_xs[tt]], scale=r_[:])
                for dc in range(DC):
                    pt = ptr[:, dc % 2, :]; bpt = B_ptr[dc % 2]
                    for tt in range(4):
                        TR(pt[:, tt * 128:(tt + 1) * 128], xs[tt][:, dc * 128:(dc + 1) * 128], ident_bf[:],
                           reads=[B_xs[tt], B_ident], writes=[bpt])
                    if dc % 2 == 0:
                        TS("dve", h[:, dc, :], pt, A1[:, dc:dc + 1], SH1[:, dc:dc + 1], ALU.mult, ALU.add,
                           reads=[bpt, B_A1, B_modT], writes=[bh])
                    else:
                        ACT(h[:, dc, :], pt, AF.Identity, reads=[bpt, B_A1, B_modT], writes=[bh],
                            scale=A1[:, dc:dc + 1], bias=SH1[:, dc:dc + 1])
                if "skip_K" in debug:
                    continue
                if "hT" in debug and blk == NC:
                    d = dbg_out("hT", [128, DC, TB], BF16); DMA("sp", d[:, :, :], h[:], reads=[bh])
                for g in range(2):
                    w, bw = load_w(OFF_K + g * 512)
                    for oc in range(4):
                        hd = g * 4 + oc
                        p = pp[ppi % 2]; bp = B_pp[ppi % 2]; ppi += 1
                        for dc in range(DC):
                            MM(p[:, :], w[:, dc, oc * 128:(oc + 1) * 128], h[:, dc, :], dc == 0, dc == DC - 1,
                               reads=[bw, bh], writes=[bp])
                        e_ = ev[evi % 3]; be = B_ev[evi % 3]; evi += 1
                        CP("act", e_[:], p[:, :], reads=[bp], writes=[be])
                        DMA("sp", KT_d[hd, :, blk * TB:(blk + 1) * TB], e_[:], reads=[be], writes=[B_KT])
                for g in range(0 if "skip_V" in debug else 2):
                    w, bw = load_w(OFF_V + g * 512)
                    for tt in range(4):
                        p = pp[ppi % 2]; bp = B_pp[ppi % 2]; ppi += 1
                        for dc in range(DC):
                            MM(p[:, :], h[:, dc, tt * 128:(tt + 1) * 128], w[:, dc, :], dc == 0, dc == DC - 1,
                               reads=[bw, bh], writes=[bp])
                        e_ = ev[evi % 3]; be = B_ev[evi % 3]; evi += 1
                        CP("dve", e_[:], p[:, :], reads=[bp], writes=[be])
                        t0 = blk * TB + tt * 128
                        DMA("sp", V_d[g * 4:(g + 1) * 4, t0:t0 + 128, :].rearrange("h t c -> t h c"),
                            e_[:].rearrange("p (h c) -> p h c", h=4), reads=[be], writes=[B_V])
                if "skip_F" in debug:
                    continue
                for dc in range(DC):
                    MM(pf[0:H, :], wf[:, dc, :], h[:, dc, :], dc == 0, dc == DC - 1, reads=[B_wf, bh], writes=[B_pf])
                ACT(fe[:], pf[0:H, :], AF.Exp, reads=[B_pf, B_nbf], writes=[B_fe], scale=-1.0, bias=nbf[:])
                ACT(fl[:], fe[:], AF.Ln, reads=[B_fe], writes=[B_fl], bias=1.0)
                nct = ncum[blk % 2]; bnc = B_ncum[blk % 2]
                init = 0.0 if fcarry is None else fcarry
                S.op("dve", lambda e, nct=nct, init=init: e.tensor_tensor_scan(
                    out=nct[:], data0=fones[:], data1=fl[:], initial=init, op0=ALU.mult, op1=ALU.add),
                    reads=[B_fones, B_fl] + ([B_ncum[(blk - 1) % 2]] if fcarry is not None else []), writes=[bnc])
                fcarry = nct[:, TB - 1:TB]
                DMA("sp", NCUM_d[:, blk * TB:(blk + 1) * TB], nct[:], reads=[bnc], writes=[B_NCUM])
                if "skip_Q" in debug:
                    continue
                if own:
                    for g in range(2):
                        w, bw = load_w(OFF_Q + g * 512)
                        for oc in range(4):
                            hd = g * 4 + oc
                            p = pp[ppi % 2]; bp = B_pp[ppi % 2]; ppi += 1
                            for dc in range(DC):
                                MM(p[:, :], w[:, dc, oc * 128:(oc + 1) * 128], h[:, dc, :], dc == 0, dc == DC - 1,
                                   reads=[bw, bh], writes=[bp])
                            e_ = ev[evi % 3]; be = B_ev[evi % 3]; evi += 1
                            S.op("act", lambda e, e_=e_, p=p: e.mul(out=e_[:], in_=p[:, :], mul=128.0 ** -0.5),
                                 reads=[bp], writes=[be])
                            DMA("sp", QT_d[hd, :, ob * TB:(ob + 1) * TB], e_[:], reads=[be], writes=[B_QT])
                    for g in range(2):
                        w, bw = load_w(OFF_LY + g * 512)
                        for oc in range(4):
                            n = g * 4 + oc
                            p = pp[ppi % 2]; bp = B_pp[ppi % 2]; ppi += 1
                            for dc in range(DC):
                                MM(p[:, :], w[:, dc, oc * 128:(oc + 1) * 128], h[:, dc, :], dc == 0, dc == DC - 1,
                                   reads=[bw, bh], writes=[bp])
                            ACT(gly[:, n, :], p[:, :], AF.Gelu_apprx_tanh, reads=[bp], writes=[B_gly[n]])
                for g in range(0 if "skip_LX" in debug else 2):
                    w, bw = load_w(OFF_LX + g * 512)
                    for oc in range(4):
                        n = g * 4 + oc
                        k2 = n % 2
                        p = pp[ppi % 2]; bp = B_pp[ppi % 2]; ppi += 1
                        for dc in range(DC):
                            MM(p[:, :], w[:, dc, oc * 128:(oc + 1) * 128], h[:, dc, :], dc == 0, dc == DC - 1,
                               reads=[bw, bh], writes=[bp])
                        lx = lxb[n]; blx = B_lxb[n]
                        if blk == NC:
                            TS("pool", lx[:, 0:3], lx[:, 0:3], ctxk[:, 0:1], None, ALU.mult, reads=[blx, B_ctxk],
                               writes=[blx])
                            TS("pool", carry[:, n:n + 1], carry[:, n:n + 1], ctxk[:, 0:1], None, ALU.mult,
                               reads=[B_carry[n], B_ctxk], writes=[B_carry[n]])
                        CP("act", lx[:, 3:3 + TB], p[:, :], reads=[bp], writes=[blx])
                        xc_ = xcv[k2]; bxc = B_xcv[k2]
                        TS("pool", xc_[:], lx[:, 0:TB], convw[:, n, 0:1], convb[:, n:n + 1], ALU.mult, ALU.add,
                           reads=[blx, B_convw, B_convb], writes=[bxc])
                        for k in range(1, 4):
                            STT("dve", xc_[:], lx[:, k:k + TB], convw[:, n, k:k + 1], xc_[:], ALU.mult, ALU.add,
                                reads=[blx, B_convw, bxc], writes=[bxc])
                        CP("pool", lx[:, 0:3], lx[:, TB:TB + 3], reads=[blx], writes=[blx])
                        CP("act", xcb[k2][:], xc_[:], reads=[bxc], writes=[B_xcb[k2]])
                        MM(pg[0][:, :], lwa[:, n, :], xcb[k2][:], True, True, reads=[B_lwa, B_xcb[k2]], writes=[B_pg[0]])
                        MM(pg[1][:, :], lwx[:, n, :], xcb[k2][:], True, True, reads=[B_lwx, B_xcb[k2]], writes=[B_pg[1]])
                        ACT(gr[k2][:], pg[0][:, :], AF.Sigmoid, reads=[B_pg[0], B_lba], writes=[B_gr[k2]],
                            bias=lba[:, n:n + 1])
                        ACT(gi[k2][:], pg[1][:, :], AF.Sigmoid, reads=[B_pg[1], B_lbx], writes=[B_gi[k2]],
                            bias=lbx[:, n:n + 1])
                        ACT(ga_[k2][:], gr[k2][:], AF.Exp, reads=[B_gr[k2], B_sca], writes=[B_ga[k2]],
                            scale=sca[:, n:n + 1])
                        ACT(gq[k2][:], gr[k2][:], AF.Exp, reads=[B_gr[k2], B_sca2], writes=[B_gq[k2]],
                            scale=sca2[:, n:n + 1])
                        ACT(gq[k2][:], gq[k2][:], AF.Sqrt, reads=[B_gq[k2]], writes=[B_gq[k2]],
                            scale=-1.0, bias=1.0 + 2.0 ** -23)
                        TTE("dve", gi[k2][:], gi[k2][:], xc_[:], ALU.mult, reads=[B_gi[k2], bxc], writes=[B_gi[k2]])
                        TTE("dve", gi[k2][:], gi[k2][:], gq[k2][:], ALU.mult, reads=[B_gi[k2], B_gq[k2]],
                            writes=[B_gi[k2]])
                        hh_ = hh[k2]; bhh = B_hh[k2]
                        S.op("dve", lambda e, hh_=hh_, k2=k2, n=n: e.tensor_tensor_scan(
                            out=hh_[:], data0=ga_[k2][:], data1=gi[k2][:], initial=carry[:, n:n + 1],
                            op0=ALU.mult, op1=ALU.add), reads=[B_ga[k2], B_gi[k2], B_carry[n]], writes=[bhh])
                        CP("dve", carry[:, n:n + 1], hh_[:, TB - 1:TB], reads=[bhh], writes=[B_carry[n]])
                        if own:
                            e_ = ev[evi % 3]; be = B_ev[evi % 3]; evi += 1
                            TTE("dve", e_[:], hh_[:], gly[:, n, :], ALU.mult, reads=[bhh, B_gly[n]], writes=[be])
                            DMA("sp", LRUT_d[n, :, ob * TB:(ob + 1) * TB], e_[:], reads=[be], writes=[B_LRUT])
                            TTE("pool", lsq[k2][:], e_[:], e_[:], ALU.mult, reads=[be], writes=[B_lsq[k2]])
                            MM(pss[0:1, :], ones_bf[:, 0:1], lsq[k2][:], n == 0, n == 7,
                               reads=[B_lsq[k2], B_onesbf], writes=[B_pss])
                if own:
                    CP("dve", ssrow[:], pss[0:1, :], reads=[B_pss], writes=[B_ssrow])
                    for tt in range(4):
                        MM(pss[:, tt:tt + 1], ssrow[0:1, tt * 128:(tt + 1) * 128], ones_row[0:1, 0:1], True, True,
                           reads=[B_ssrow, B_ones], writes=[B_pss])
                    CP("dve", ssl[:], pss[:, 0:4], reads=[B_pss], writes=[B_ssl])
                    RSTD(ssl[:], rs_l[:, ob * 4:(ob + 1) * 4], 1024, B_ssl, B_rsl)
            if "p1k" in debug:
                d = dbg_out("KT", [H, 128, TT], BF16); DMA("sp", d[:, :, :], KT_d[:, :, :], reads=[B_KT])
            if "p1" in debug:
                d = dbg_out("rs_l", [128, NO * 4]); DMA("sp", d[:, :], rs_l[:], reads=[B_rsl])
                d = dbg_out("KT", [H, 128, TT], BF16); DMA("sp", d[:, :, :], KT_d[:, :, :], reads=[B_KT])
                d = dbg_out("V", [H, TT, 128], BF16); DMA("sp", d[:, :, :], V_d[:, :, :], reads=[B_V])
                d = dbg_out("QT", [H, 128, TO], BF16); DMA("sp", d[:, :, :], QT_d[:, :, :], reads=[B_QT])
                d = dbg_out("NCUM", [H, TT]); DMA("sp", d[:, :], NCUM_d[:, :], reads=[B_NCUM])
                d = dbg_out("LRUT", [8, 128, TO], BF16); DMA("sp", d[:, :, :], LRUT_d[:, :, :], reads=[B_LRUT])
            S.emit_phase()
        if stop_after <= 1:
            return nc, dbg

        with ExitStack() as st:
            wo = sbt(st, "wo", [128, DC, D], BF16); B_wo = bufs(DC)
            gmrg = sbt(st, "gmrg", [128, DC], F32); B_gmrg = Buf()
            onehot = sbt(st, "onehot_t", [96, H, 128], BF16); B_oh = Buf()
            maskb = [sbt(st, "maskb%d" % r, [128, TB], BF16) for r in range(4)]; B_mask = bufs(4)
            ncol = sbt(st, "ncol", [128, NKT, H], F32); B_ncol = Buf()
            Rb = sbt(st, "Rb", [128, H], F32); B_Rb = Buf()
            bias_all = sbt(st, "bias_all", [128, NKT, H], F32); B_bias = Buf()
            X96 = sbt(st, "X96", [96, TB], F32); B_X96 = Buf()
            R96 = sbt(st, "R96", [96, 1], F32); B_R96 = Buf()
            R1 = sbt(st, "R1", [96, TB], F32); B_R1 = Buf()
            HI = sbt(st, "HI", [96, TB], BF16); B_HI = Buf()
            QA = sbt(st, "QA", [96, TB], BF16); B_QA = Buf()
            qT = [sbt(st, "qT%d" % i, [128, TB], BF16) for i in range(2)]; B_qT = bufs(2)
            kT = [sbt(st, "kT%d" % i, [128, TT], BF16) for i in range(2)]; B_kT = bufs(2)
            va = [sbt(st, "va%d" % i, [128, NKT, 129], BF16) for i in range(2)]; B_va = bufs(2)
            pT = [sbt(st, "pT%d" % i, [128, TB], BF16) for i in range(3)]; B_pT = bufs(3)
            rden = sbt(st, "rden", [128, 4], F32); B_rden = bufs(4)
            attn = [sbt(st, "attn%d" % i, [128, 1024], BF16) for i in range(4)]; B_attn = bufs(4)
            ssa = sbt(st, "ssa", [128, 4], F32); B_ssa = bufs(4)
            rs_a = sbt(st, "rs_a", [128, 4], F32); B_rsa = bufs(4)
            attnT = sbt(st, "attnT", [128, 8, TB], BF16); B_attnT = Buf()
            lruT = sbt(st, "lruT", [128, 8, TB], BF16); B_lruT = Buf()
            xt2 = sbt(st, "xt2", [128, D], F32); B_xt2 = Buf()
            x1t = sbt(st, "x1t", [128, D], F32); B_x1t = Buf()
            xs2 = sbt(st, "xs2", [128, D], BF16); B_xs2 = Buf()
            junk2 = sbt(st, "junk2", [128, D], BF16); B_junk2 = Buf()
            ss2 = sbt(st, "ss2", [128, 1], F32); B_ss2 = Buf()
            rstd2 = sbt(st, "rstd2", [128, 1], F32); B_rstd2 = Buf()
            h2s = [sbt(st, "h2s%d" % i, [128, DC, 128], BF16) for i in range(2)]; B_h2s = bufs(2)
            bank = [pst(st, "bank%d" % i, [128, 512], F32) for i in range(8)]; B_bank = bufs(8)
            pS, B_pS = bank[0:2], B_bank[0:2]
            pO, B_pO = bank[2:6], B_bank[2:6]
            pW, B_pW = [bank[6], bank[7], bank[0], bank[1]], [B_bank[6], B_bank[7], B_bank[0], B_bank[1]]

            DMA("sp", gmrg[:], gmrg_d[:, :], writes=[B_gmrg])
            for q4 in range(4):
                DMA("pool", wo[:, q4 * 4:(q4 + 1) * 4, :],
                    wout_d[q4 * 512:(q4 + 1) * 512, :].rearrange("(kc p) d -> p kc d", p=128),
                    writes=B_wo[q4 * 4:(q4 + 1) * 4])
            for kc in range(DC):
                STT("dve", wo[:, kc, :], wo[:, kc, :], gmrg[:, kc:kc + 1], GA1b[:], ALU.mult, ALU.mult,
                    reads=[B_wo[kc], B_gmrg, B_GA1b], writes=[B_wo[kc]])
            DMA("pool", onehot[:], onehot_d[:, :, :], writes=[B_oh])
            for r in range(4):
                MEMSET("pool", maskb[r][:], 0.0, writes=[B_mask[r]])
                S.op("pool", lambda e, r=r: e.affine_select(
                    out=maskb[r][:], in_=maskb[r][:], pattern=[[1, TB]], compare_op=ALU.is_ge, fill=NEG,
                    base=-r * 128, channel_multiplier=-1), reads=[B_mask[r]], writes=[B_mask[r]])
            for hh_ in range(H):
                for half in range(2):
                    k0, k1 = half * NKT // 2, (half + 1) * NKT // 2
                    DMA("sp", ncol[:, k0:k1, hh_:hh_ + 1],
                        NCUM_d[hh_:hh_ + 1, k0 * 128:k1 * 128].rearrange("o (kt p) -> p kt o", p=128),
                        reads=[B_NCUM], writes=[B_ncol], slow=True)
            MEMSET("pool", X96[:], 0.0, writes=[B_X96])
            MEMSET("pool", R96[:], 0.0, writes=[B_R96])
            MEMSET("pool", QA[:], 0.0, writes=[B_QA])
            for i in range(2):
                MEMSET("pool", va[i][:, :, 128:129], 1.0, writes=[B_va[i]])
            hi_ = 0
            pti = 0
            for qb in range(NO):
                q0 = TC + qb * TB
                L = q0 + TB
                nkt = L // 128
                kt0 = q0 // 128
                DMA("sp", Rb[:], NCUM_d[:, q0:q0 + 1].rearrange("h o -> o h").partition_broadcast(128),
                    reads=[B_NCUM], writes=[B_Rb], slow=True)
                for g3 in range(3):
                    DMA("sp", R96[g3 * 32:g3 * 32 + H, :], NCUM_d[:, q0:q0 + 1], reads=[B_NCUM], writes=[B_R96],
                        slow=True)
                    DMA("sp", X96[g3 * 32:g3 * 32 + H, :], NCUM_d[:, q0:q0 + TB], reads=[B_NCUM], writes=[B_X96])
                TTE("dve", bias_all[:, 0:nkt, :], ncol[:, 0:nkt, :], Rb[:].unsqueeze(1).to_broadcast([128, nkt, H]),
                    ALU.subtract, reads=[B_ncol, B_Rb], writes=[B_bias])
                if TC > 0:
                    TS("dve", bias_all[:, 0:TC // 128, :], bias_all[:, 0:TC // 128, :], ctxb[:, 0:1], None, ALU.add,
                       reads=[B_bias, B_ctxb], writes=[B_bias])
                TS("dve", X96[:], X96[:], R96[:, 0:1], -1.0, ALU.subtract, ALU.mult, reads=[B_X96, B_R96],
                   writes=[B_X96])
                CP("dve", HI[:], X96[:], reads=[B_X96], writes=[B_HI])
                CP("dve", QA[0:H, :], HI[0:H, :], reads=[B_HI], writes=[B_QA])
                TTE("dve", R1[:], X96[:], HI[:], ALU.subtract, reads=[B_X96, B_HI], writes=[B_R1])
                CP("dve", HI[:], R1[:], reads=[B_R1], writes=[B_HI])
                CP("dve", QA[32:32 + H, :], HI[32:32 + H, :], reads=[B_HI], writes=[B_QA])
                TTE("dve", R1[:], R1[:], HI[:], ALU.subtract, reads=[B_R1, B_HI], writes=[B_R1])
                CP("dve", QA[64:64 + H, :], R1[64:64 + H, :], reads=[B_R1], writes=[B_QA])
                for h in range(H):
                    q_ = qT[hi_ % 2]; bq = B_qT[hi_ % 2]
                    k_ = kT[hi_ % 2]; bk = B_kT[hi_ % 2]
                    v_ = va[hi_ % 2]; bv = B_va[hi_ % 2]
                    hi_ += 1
                    DMA("sp", q_[:], QT_d[h, :, qb * TB:(qb + 1) * TB], reads=[B_QT], writes=[bq])
                    DMA("sp", k_[:, 0:L], KT_d[h, :, 0:L], reads=[B_KT], writes=[bk])
                    DMA("sp", v_[:, 0:nkt, 0:128], V_d[h, 0:L, :].rearrange("(kt p) c -> p kt c", p=128),
                        reads=[B_V], writes=[bv])
                    def emit_S(kt):
                        diag = kt >= kt0
                        r = kt - kt0
                        ps = pS[kt % 2]; bps = B_pS[kt % 2]
                        MM(ps[:, :], k_[:, kt * 128:(kt + 1) * 128], q_[:], True, False, reads=[bk, bq], writes=[bps])
                        MM(ps[:, :], onehot[:, h, :], QA[:], False, not diag, reads=[B_oh, B_QA], writes=[bps])
                        if diag:
                            MM(ps[:, :], ident_bf[:], maskb[r][:], False, True, reads=[B_ident, B_mask[r]],
                               writes=[bps])

                    emit_S(0)
                    for kt in range(nkt):
                        diag = kt >= kt0
                        r = kt - kt0
                        ps = pS[kt % 2]; bps = B_pS[kt % 2]
                        p_ = pT[pti % 3]; bp_ = B_pT[pti % 3]; pti += 1
                        ACT(p_[:], ps[:, :], AF.Exp, reads=[bps, B_bias], writes=[bp_], bias=bias_all[:, kt, h:h + 1])
                        if kt + 1 < nkt:
                            emit_S(kt + 1)
                        for sub in range(4):
                            if diag and sub < r:
                                continue
                            MM(pO[sub][:, 0:129], p_[:, sub * 128:(sub + 1) * 128], v_[:, kt, :], kt == 0,
                               kt == kt0 + sub, reads=[bp_, bv], writes=[B_pO[sub]])
                    for sub in range(4):
                        S.op("dve", lambda e, sub=sub: e.reciprocal(out=rden[:, sub:sub + 1], in_=pO[sub][:, 128:129]),
                             reads=[B_pO[sub]], writes=[B_rden[sub]])
                        ACT(attn[sub][:, h * 128:(h + 1) * 128], pO[sub][:, 0:128], AF.Copy,
                            reads=[B_pO[sub], B_rden[sub]], writes=[B_attn[sub]], scale=rden[:, sub:sub + 1])
                for sub in range(4):
                    ACT(junk2[:, 0:1024], attn[sub][:], AF.Square, reads=[B_attn[sub]], writes=[B_junk2, B_ssa[sub]],
                        accum=ssa[:, sub:sub + 1])
                    RSTD(ssa[:, sub:sub + 1], rs_a[:, sub:sub + 1], 1024, B_ssa[sub], B_rsa[sub])
                for kc in range(8):
                    pw = pW[kc % 4]; bpw = B_pW[kc % 4]
                    for sub in range(4):
                        TR(pw[:, sub * 128:(sub + 1) * 128], attn[sub][:, kc * 128:(kc + 1) * 128], ident_bf[:],
                           reads=[B_attn[sub], B_ident], writes=[bpw])
                    CP("act" if kc % 2 else "dve", attnT[:, kc, :], pw[:, :], reads=[bpw], writes=[B_attnT])
                DMA("sp", lruT[:], LRUT_d[:, :, qb * TB:(qb + 1) * TB].rearrange("n p t -> p n t"),
                    reads=[B_LRUT], writes=[B_lruT])
                pwi = 0
                for sub in range(4):
                    t0 = qb * TB + sub * 128
                    DMA("sp", xt2[:], xo_d[t0:t0 + 128, :], writes=[B_xt2])
                    for dt in range(4):
                        pa = pW[pwi % 4]; bpa = B_pW[pwi % 4]; pwi += 1
                        pl = pW[pwi % 4]; bpl = B_pW[pwi % 4]; pwi += 1
                        for kc in range(8):
                            MM(pa[:, :], attnT[:, kc, sub * 128:(sub + 1) * 128], wo[:, kc, dt * 512:(dt + 1) * 512],
                               kc == 0, kc == 7, reads=[B_attnT, B_wo[kc]], writes=[bpa])
                        for kc in range(8):
                            MM(pl[:, :], lruT[:, kc, sub * 128:(sub + 1) * 128],
                               wo[:, 8 + kc, dt * 512:(dt + 1) * 512], kc == 0, kc == 7,
                               reads=[B_lruT, B_wo[8 + kc]], writes=[bpl])
                        STT("dve", x1t[:, dt * 512:(dt + 1) * 512], pa[:, :], rs_a[:, sub:sub + 1],
                            xt2[:, dt * 512:(dt + 1) * 512], ALU.mult, ALU.add,
                            reads=[bpa, B_rsa[sub], B_xt2], writes=[B_x1t])
                        STT("dve", x1t[:, dt * 512:(dt + 1) * 512], pl[:, :], rs_l[:, qb * 4 + sub:qb * 4 + sub + 1],
                            x1t[:, dt * 512:(dt + 1) * 512], ALU.mult, ALU.add,
                            reads=[bpl, B_rsl, B_x1t], writes=[B_x1t])
                    DMA("sp", X1_d[t0:t0 + 128, :], x1t[:], reads=[B_x1t], writes=[B_X1])
                    ACT(junk2[:], x1t[:], AF.Square, reads=[B_x1t], writes=[B_junk2, B_ss2], accum=ss2[:])
                    RSTD(ss2[:], rstd2[:], D, B_ss2, B_rstd2)
                    ACT(xs2[:], x1t[:], AF.Copy, reads=[B_x1t, B_rstd2], writes=[B_xs2], scale=rstd2[:])
                    h2 = h2s[sub % 2]; bh2 = B_h2s[sub % 2]
                    for g4 in range(4):
                        po = pO[g4]; bpo = B_pO[g4]
                        for j in range(4):
                            dc = g4 * 4 + j
                            TR(po[:, j * 128:(j + 1) * 128], xs2[:, dc * 128:(dc + 1) * 128], ident_bf[:],
                               reads=[B_xs2, B_ident], writes=[bpo])
                        for j in range(4):
                            dc = g4 * 4 + j
                            if j % 2 == 0:
                                TS("dve", h2[:, dc, :], po[:, j * 128:(j + 1) * 128], A2[:, dc:dc + 1],
                                   SH2[:, dc:dc + 1], ALU.mult, ALU.add, reads=[bpo, B_A2, B_modT], writes=[bh2])
                            else:
                                ACT(h2[:, dc, :], po[:, j * 128:(j + 1) * 128], AF.Identity, reads=[bpo, B_A2, B_modT],
                                    writes=[bh2], scale=A2[:, dc:dc + 1], bias=SH2[:, dc:dc + 1])
                    DMA("sp", H2T_d[:, :, t0:t0 + 128], h2[:], reads=[bh2], writes=[B_H2T])
            if "p2" in debug:
                d = dbg_out("X1", [TO, D]); DMA("sp", d[:, :], X1_d[:, :], reads=[B_X1])
                d = dbg_out("H2T", [128, DC, TO], BF16); DMA("sp", d[:, :, :], H2T_d[:, :, :], reads=[B_H2T])
            S.emit_phase()
        if stop_after <= 2:
            return nc, dbg
        mid.close()

        with ExitStack() as st4:
            E1 = [sbt(st4, "E1_%d" % i, [128, H, 128], F32) for i in range(4)]; B_E1 = bufs(4)
            E2 = [sbt(st4, "E2_%d" % i, [128, H, 128], F32) for i in range(4)]; B_E2 = bufs(4)
            kap = sbt(st4, "kap", [128, 4, H], F32); B_kap = bufs(4)
            nkapB = sbt(st4, "nkapB", [128, 4, H], F32)
            h2T = sbt(st4, "h2T", [128, DC, TB], BF16); B_h2T = Buf()
            bank = [pst(st4, "pbank%d" % i, [128, 512], F32) for i in range(8)]; B_bank = bufs(8)
            for ob in range(NO):
                c0_ = ob * TB
                DMA("sp", h2T[:], H2T_d[:, :, c0_:c0_ + TB], reads=[B_H2T], writes=[B_h2T])
                with ExitStack() as st:
                    k1t = sbt(st, "k1t", [128, H, 128], BF16); B_k1t = Buf()
                    k2t = sbt(st, "k2t", [128, H, 128], BF16); B_k2t = Buf()
                    wqb = [sbt(st, "wqb%d" % i, [128, DC, 512], BF16) for i in range(2)]; B_wqb = bufs(2)
                    qp = sbt(st, "qp", [128, 16, TB], BF16); B_qp = bufs(16)
                    s12 = [sbt(st, "s12_%d" % i, [128, H, 128], F32) for i in range(2)]; B_s12 = bufs(2)
                    wk = sbt(st, "wk", [128, 256], F32); B_wk = Buf()
                    wk2 = sbt(st, "wk2", [128, 256], F32); B_wk2 = Buf()
                    v12 = [sbt(st, "v12_%d" % i, [128, H, 16], F32) for i in range(2)]; B_v12 = bufs(2)
                    cand = sbt(st, "cand", [128, H, 16, 16], F32); B_cand = Buf()
                    ctop = sbt(st, "ctop", [128, H, 24], F32); B_ctop = Buf()
                    tau = sbt(st, "tau", [128, H], F32); B_tau = Buf()
                    ez = sbt(st, "ez", [128, H, 16], F32); B_ez = Buf()
                    zs = sbt(st, "zs", [128, H], F32); B_zs = Buf()
                    DMA("pool", k1t[:], k1t_d[:, :, :], writes=[B_k1t])
                    DMA("pool", k2t[:], k2t_d[:, :, :], writes=[B_k2t])
                    bi = 0
                    for g in range(4):
                        w = wqb[g % 2]; bw = B_wqb[g % 2]
                        DMA("pool", w[:], wq_d[:, g * 512:(g + 1) * 512].rearrange("(dc p) c -> p dc c", p=128),
                            writes=[bw])
                        for oc in range(4):
                            j = g * 4 + oc
                            p = bank[bi % 4]; bp = B_bank[bi % 4]; bi += 1
                            for dc in range(DC):
                                MM(p[:, :], w[:, dc, oc * 128:(oc + 1) * 128], h2T[:, dc, :], dc == 0, dc == DC - 1,
                                   reads=[bw, B_h2T], writes=[bp])
                            CP("act" if j % 2 else "dve", qp[:, j, :], p[:, :], reads=[bp], writes=[B_qp[j]])
                    for tt in range(4):
                        tsl = slice(tt * 128, (tt + 1) * 128)
                        for si, kt_ in ((0, k1t), (1, k2t)):
                            for h in range(H):
                                b_ = 4 + si * 2 + h // 4
                                MM(bank[b_][:, (h % 4) * 128:(h % 4 + 1) * 128], qp[:, 2 * h + si, tsl], kt_[:, h, :],
                                   True, True, reads=[B_qp[2 * h + si], B_k1t, B_k2t], writes=[B_bank[b_]])
                            for hb in range(2):
                                b_ = 4 + si * 2 + hb
                                CP("act", s12[si][:, hb * 4:(hb + 1) * 4, :].rearrange("p h n -> p (h n)"),
                                   bank[b_][:, :], reads=[B_bank[b_]], writes=[B_s12[si]])
                            for h in range(H):
                                S.op("dve", lambda e, si=si, h=h: e.max(out=v12[si][:, h, 0:8], in_=s12[si][:, h, :]),
                                     reads=[B_s12[si]], writes=[B_v12[si]])
                                S.op("dve", lambda e, si=si, h=h: e.match_replace(
                                    out=wk[:, 0:128], in_to_replace=v12[si][:, h, 0:8], in_values=s12[si][:, h, :],
                                    imm_value=-1e30), reads=[B_s12[si], B_v12[si]], writes=[B_wk])
                                S.op("dve", lambda e, si=si, h=h: e.max(out=v12[si][:, h, 8:16], in_=wk[:, 0:128]),
                                     reads=[B_wk], writes=[B_v12[si]])
                        v1, v2 = v12
                        TTE("pool", cand[:], v1[:].unsqueeze(3).to_broadcast([128, H, 16, 16]),
                            v2[:].unsqueeze(2).to_broadcast([128, H, 16, 16]), ALU.add,
                            reads=[B_v12[0], B_v12[1]], writes=[B_cand])
                        for h in range(H):
                            cf = cand[:, h, :, :].rearrange("p a b -> p (a b)")
                            S.op("dve", lambda e, h=h, cf=cf: e.max(out=ctop[:, h, 0:8], in_=cf),
                                 reads=[B_cand], writes=[B_ctop])
                            S.op("dve", lambda e, h=h, cf=cf: e.match_replace(
                                out=wk[:], in_to_replace=ctop[:, h, 0:8], in_values=cf, imm_value=-1e30),
                                reads=[B_cand, B_ctop], writes=[B_wk])
                            S.op("dve", lambda e, h=h: e.max(out=ctop[:, h, 8:16], in_=wk[:]),
                                 reads=[B_wk], writes=[B_ctop])
                            S.op("dve", lambda e, h=h: e.match_replace(
                                out=wk2[:], in_to_replace=ctop[:, h, 8:16], in_values=wk[:], imm_value=-1e30),
                                reads=[B_wk, B_ctop], writes=[B_wk2])
                            S.op("dve", lambda e, h=h: e.max(out=ctop[:, h, 16:24], in_=wk2[:]),
                                 reads=[B_wk2], writes=[B_ctop])
                        TTE("dve", tau[:], ctop[:, :, 15], ctop[:, :, 16], ALU.add, reads=[B_ctop], writes=[B_tau])
                        TTE("dve", ez[:], ctop[:, :, 0:16], ctop[:, :, 0:1].to_broadcast([128, H, 16]), ALU.subtract,
                            reads=[B_ctop], writes=[B_ez])
                        ACT(ez[:], ez[:], AF.Exp, reads=[B_ez], writes=[B_ez])
                        S.op("dve", lambda e: e.reduce_sum(out=zs[:], in_=ez[:], axis=mybir.AxisListType.X),
                             reads=[B_ez], writes=[B_zs])
                        S.op("dve", lambda e: e.reciprocal(out=zs[:], in_=zs[:]), reads=[B_zs], writes=[B_zs])
                        STT("dve", tau[:], tau[:], 0.5, ctop[:, :, 0], ALU.mult, ALU.subtract, reads=[B_tau, B_ctop],
                            writes=[B_tau])
                        ACT(tau[:], tau[:], AF.Exp, reads=[B_tau], writes=[B_tau])
                        TTE("dve", kap[:, tt, :], tau[:], zs[:], ALU.mult, reads=[B_tau, B_zs], writes=[B_kap[tt]])
                        TS("dve", nkapB[:, tt, :], kap[:, tt, :], -BIGM, None, ALU.mult, reads=[B_kap[tt]],
                           writes=[B_kap[tt]])
                        TTE("pool", s12[0][:], s12[0][:], v1[:, :, 0:1].to_broadcast([128, H, 128]), ALU.subtract,
                            reads=[B_s12[0], B_v12[0]], writes=[B_s12[0]])
                        ACT(s12[0][:], s12[0][:], AF.Exp, reads=[B_s12[0]], writes=[B_s12[0]])
                        TTE("pool", E1[tt][:], s12[0][:], zs[:].unsqueeze(2).to_broadcast([128, H, 128]), ALU.mult,
                            reads=[B_s12[0], B_zs], writes=[B_E1[tt]])
                        TTE("pool", s12[1][:], s12[1][:], v2[:, :, 0:1].to_broadcast([128, H, 128]), ALU.subtract,
                            reads=[B_s12[1], B_v12[1]], writes=[B_s12[1]])
                        ACT(E2[tt][:], s12[1][:], AF.Exp, reads=[B_s12[1]], writes=[B_E2[tt]])
                    if "p4a" in debug and ob == 0:
                        d = dbg_out("E1", [128, H, 128]); DMA("sp", d[:, :, :], E1[0][:], reads=[B_E1[0]])
                        d = dbg_out("E2", [128, H, 128]); DMA("sp", d[:, :, :], E2[0][:], reads=[B_E2[0]])
                        d = dbg_out("kap", [128, 4, H]); DMA("sp", d[:, :, :], kap[:], reads=B_kap)
                    S.emit_phase()
                with ExitStack() as sacc:
                  acc = [sbt(sacc, "acc%d" % i, [128, D], F32) for i in range(4)]; B_acc = bufs(4)
                  with ExitStack() as st:
                    uT = [sbt(st, "uT%d" % i, [128, DC, GE], BF16) for i in range(2)]; B_uT = bufs(2)
                    vg = [sbt(st, "vg%d" % i, [128, 4, D], BF16) for i in range(2)]; B_vg = bufs(2)
                    gT = [sbt(st, "gT%d" % i, [128, 4, TB], BF16) for i in range(2)]
                    B_gT = [bufs(4) for _ in range(2)]
                    AT = [sbt(st, "AT%d" % i, [128, 4, TB], BF16) for i in range(2)]
                    B_AT = [bufs(4) for _ in range(2)]
                    Xh = [sbt(st, "Xh%d" % i, [128, 2, 4, 128], F32) for i in range(4)]; B_Xh = bufs(4)
                    Yb = [sbt(st, "Yb%d" % i, [128, 2, 4, 128], BF16) for i in range(2)]; B_Yb = bufs(2)
                    print("stage B sbuf remaining", nc.sbuf_bytes_remaining)
                    G = [sbt(st, "G%d" % i, [128, H, 4, 128], BF16) for i in range(2)]; B_G = bufs(2)
                    B_accd = [bufs(4) for _ in range(4)]
                    pz, B_pz = bank[0:2], B_bank[0:2]
                    pw_, B_pw_ = bank[2:4], B_bank[2:4]
                    py, B_py = bank[4:8], B_bank[4:8]
                    ctr = {"x": 0, "y": 0}
                    ngroups = NG if "fewgroups" not in debug else 2

                    def load_u(g):
                        DMA("pool", uT[g % 2][:], ut_d[:, g * GE:(g + 1) * GE].rearrange("(dc p) e -> p dc e", p=128),
                            writes=[B_uT[g % 2]])

                    def load_v(g):
                        DMA("pool", vg[g % 2][:], vt_d[g * GE:(g + 1) * GE, :].rearrange("(c p) d -> p c d", p=128),
                            writes=[B_vg[g % 2]])

                    def emit_z(g):
                        u_ = uT[g % 2]; bu = B_uT[g % 2]
                        for c in range(4):
                            p = pz[c % 2]; bp = B_pz[c % 2]
                            for dc in range(DC):
                                MM(p[:, :], u_[:, dc, c * 128:(c + 1) * 128], h2T[:, dc, :], dc == 0, dc == DC - 1,
                                   reads=[bu, B_h2T], writes=[bp])
                            ACT(gT[g % 2][:, c, :], p[:, :], AF.Gelu_apprx_tanh, reads=[bp], writes=[B_gT[g % 2][c]])

                    def emit_XG(g, tt):
                        k = (g * 4 + tt) % 2
                        G_ = G[k]; bG = B_G[k]
                        xs = {}
                        for hq in (2, 3, 0, 1):
                            xi = ctr["x"]; ctr["x"] += 1
                            X_ = Xh[xi % 4]; bX = B_Xh[xi % 4]
                            xs[hq] = (X_, bX)
                            TTE("pool", X_[:],
                                E2[tt][:, 2 * hq:2 * hq + 2, :].unsqueeze(2).to_broadcast([128, 2, 4, 128]),
                                E1[tt][:, 2 * hq:2 * hq + 2, g * 4:(g + 1) * 4].unsqueeze(3).to_broadcast(
                                    [128, 2, 4, 128]), ALU.mult, reads=[B_E1[tt], B_E2[tt]], writes=[bX])
                            for h2_ in range(2):
                                h = 2 * hq + h2_
                                if hq >= 2:
                                    ACT(Yb[hq - 2][:, h2_, :, :], X_[:, h2_, :, :], AF.Relu, reads=[bX, B_kap[tt]],
                                        writes=[B_Yb[hq - 2]], scale=BIGM, bias=nkapB[:, tt, h:h + 1])
                                else:
                                    STT("dve", G_[:, h, :, :], X_[:, h2_, :, :], kap[:, tt, h:h + 1],
                                        X_[:, h2_, :, :], ALU.is_ge, ALU.mult, reads=[bX, B_kap[tt]], writes=[bG])
                        for hq in (2, 3):
                            X_, bX = xs[hq]
                            for h2_ in range(2):
                                h = 2 * hq + h2_
                                TTE("dve", G_[:, h, :, :], X_[:, h2_, :, :], Yb[hq - 2][:, h2_, :, :], ALU.min,
                                    reads=[bX, B_Yb[hq - 2]], writes=[bG])

                    def emit_hs(g, tt):
                        k = (g * 4 + tt) % 2
                        G_ = G[k]; bG = B_G[k]
                        pw = pw_[k]; bpw = B_pw_[k]
                        for c in range(4):
                            for h in range(H):
                                MM(pw[:, c * 128:(c + 1) * 128], G_[:, h, c, :], ident_bf[:], h == 0, h == H - 1,
                                   reads=[bG, B_ident], writes=[bpw])

                    def emit_AT(g, tt):
                        k = (g * 4 + tt) % 2
                        tsl = slice(tt * 128, (tt + 1) * 128)
                        TTE("dve", AT[g % 2][:, :, tsl], gT[g % 2][:, :, tsl],
                            pw_[k][:, :].rearrange("p (c t) -> p c t", c=4), ALU.mult,
                            reads=B_gT[g % 2] + [B_pw_[k]], writes=[B_AT[g % 2][tt]])

                    def emit_y(g, tt):
                        tsl = slice(tt * 128, (tt + 1) * 128)
                        a_ = AT[g % 2]; v_ = vg[g % 2]
                        for dt in range(4):
                            yi = ctr["y"]; ctr["y"] += 1
                            p = py[yi % 4]; bp = B_py[yi % 4]
                            for c in range(4):
                                MM(p[:, :], a_[:, c, tsl], v_[:, c, dt * 512:(dt + 1) * 512], c == 0, c == 3,
                                   reads=[B_AT[g % 2][tt], B_vg[g % 2]], writes=[bp])
                            dsl = slice(dt * 512, (dt + 1) * 512)
                            if g == 0:
                                CP("act", acc[tt][:, dsl], p[:, :], reads=[bp], writes=[B_accd[tt][dt]])
                            else:
                                TTE("dve", acc[tt][:, dsl], acc[tt][:, dsl], p[:, :], ALU.add,
                                    reads=[bp, B_accd[tt][dt]], writes=[B_accd[tt][dt]])

                    load_u(0)
                    load_v(0)
                    emit_XG(0, 0)
                    for g in range(ngroups + 1):
                        if g < ngroups:
                            emit_z(g)
                            if g + 1 < ngroups:
                                load_u(g + 1)
                        for tt in range(4):
                            ng_, nt_ = (g, tt + 1) if tt < 3 else (g + 1, 0)
                            if ng_ < ngroups:
                                emit_XG(ng_, nt_)
                            if g >= 1:
                                emit_y(g - 1, tt)
                            if g < ngroups:
                                emit_hs(g, tt)
                                emit_AT(g, tt)
                        if g + 1 < ngroups:
                            load_v(g + 1)
                    B_acc = [Buf() for _ in range(4)]
                    for tt in range(4):
                        S.op("pool", lambda e: e.memset(dummy[0:1, 0:1], 0.0),
                             reads=B_accd[tt], writes=[B_acc[tt], B_dummy])
                    if "p4y" in debug and ob == 0:
                        d = dbg_out("yacc", [4, 128, D])
                        for tt in range(4):
                            DMA("sp", d[tt, :, :], acc[tt][:], reads=[B_acc[tt]])
                    S.emit_phase()
                  if True:
                    with ExitStack() as stc:
                        xr = sbt(stc, "xr", [128, D], F32); B_xr = Buf()
                        junk3 = sbt(stc, "junk3", [128, D], BF16); B_junk3 = Buf()
                        GFb = sbt(stc, "GFb", [128, D], F32); B_GFb = Buf()
                        sso = sbt(stc, "sso", [128, 1], F32); B_sso = Buf()
                        rso = sbt(stc, "rso", [128, 1], F32); B_rso = Buf()
                        DMA("sp", GFb[:], gfin_d[0:1, :].partition_broadcast(128), writes=[B_GFb])
                        for tt in range(4):
                            t0 = ob * TB + tt * 128
                            DMA("sp", xr[:], X1_d[t0:t0 + 128, :], reads=[B_X1], writes=[B_xr])
                            TTE("pool", acc[tt][:], acc[tt][:], GA2b[:], ALU.mult, reads=[B_acc[tt], B_GA2b],
                                writes=[B_acc[tt]])
                            TTE("dve", acc[tt][:], acc[tt][:], xr[:], ALU.add, reads=[B_acc[tt], B_xr],
                                writes=[B_acc[tt]])
                            ACT(junk3[:], acc[tt][:], AF.Square, reads=[B_acc[tt]], writes=[B_junk3, B_sso],
                                accum=sso[:])
                            RSTD(sso[:], rso[:], D, B_sso, B_rso)
                            STT("dve", acc[tt][:], acc[tt][:], rso[:, 0:1], GFb[:], ALU.mult, ALU.mult,
                                reads=[B_acc[tt], B_rso, B_GFb], writes=[B_acc[tt]])
                            DMA("sp", out_d[t0:t0 + 128, :], acc[tt][:], reads=[B_acc[tt]])
                        S.emit_phase()
    return nc, dbg


def _col(v, n):
    return np.ascontiguousarray(np.asarray(v, np.float32).reshape(n, 128).T)


def make_in_maps(inp, NC, NO, cores):
    f = lambda a: np.ascontiguousarray(np.asarray(a, np.float32))
    shared = {
        "w_ada": f(inp["w_ada"][0]),
        "b_ada": f(inp["b_ada"][0]).reshape(1, -1),
        "g_mix_col": _col(inp["g_mix"][0], DC),
        "g_ffn_col": _col(inp["g_ffn"][0], DC),
        "w_in": f(inp["w_in"][0]),
        "nb_f_col": f(-np.asarray(inp["b_f"][0], np.float32).reshape(H, 1)),
        "conv_w_col": np.ascontiguousarray(np.asarray(inp["conv_w"][0], np.float32).reshape(4, 8, 128).transpose(2, 1, 0)),
        "conv_b_col": _col(inp["conv_b"][0], 8),
        "lru_b_a_col": _col(inp["lru_b_a"][0], 8),
        "lru_b_x_col": _col(inp["lru_b_x"][0], 8),
        "lru_lam_col": _col(inp["lru_lambda"][0], 8),
        "lru_w_a": np.ascontiguousarray(np.asarray(inp["lru_w_a"][0], np.float32).transpose(1, 0, 2)),
        "lru_w_x": np.ascontiguousarray(np.asarray(inp["lru_w_x"][0], np.float32).transpose(1, 0, 2)),
        "g_mrg_col": _col(np.concatenate([np.asarray(inp["g_attn_out"][0]), np.asarray(inp["g_lru_out"][0])]), DC),
        "w_out": f(inp["w_out"][0]),
        "peer_w_q": f(inp["peer_w_q"][0]),
        "k1t": np.ascontiguousarray(np.asarray(inp["peer_k1"][0], np.float32).transpose(2, 0, 1)),
        "k2t": np.ascontiguousarray(np.asarray(inp["peer_k2"][0], np.float32).transpose(2, 0, 1)),
        "peer_ut": np.ascontiguousarray(np.asarray(inp["peer_u"][0], np.float32).T),
        "peer_v": f(inp["peer_v"][0]),
        "g_final": f(inp["g_final"]).reshape(1, -1),
    }
    oh = np.zeros((96, H, 128), np.float32)
    for h in range(H):
        for gq in range(3):
            oh[32 * gq + h, h, :] = 1.0
    shared["onehot"] = oh
    x = np.asarray(inp["x"], np.float32)
    c = np.asarray(inp["c"], np.float32)
    maps = []
    for (b, ctx, own) in cores:
        m = dict(shared)
        if ctx is None:
            m["xc"] = np.zeros((NC * TB, D), np.float32)
            m["ctxb"] = np.full((128, 1), NEG, np.float32)
            m["ctxk"] = np.zeros((128, 1), np.float32)
        else:
            m["xc"] = np.ascontiguousarray(x[b, ctx])
            m["ctxb"] = np.zeros((128, 1), np.float32)
            m["ctxk"] = np.ones((128, 1), np.float32)
        m["xo"] = np.ascontiguousarray(x[b, own])
        m["c_col"] = _col(c[b], DC)
        maps.append(m)
    return maps


_NC_CACHE = {}


def kernel(**inputs):
    NC, NO = 4, 4
    x = np.asarray(inputs["x"])
    B, SEQ, _ = x.shape
    half = SEQ // 2
    cores = []
    for b in range(B):
        cores.append((b, None, slice(0, half)))
        cores.append((b, slice(0, half), slice(half, SEQ)))
    maps = make_in_maps(inputs, NC, NO, cores)
    if "nc" not in _NC_CACHE:
        _NC_CACHE["nc"] = build_nc(NC, NO)[0]
    res = run_bass_kernel_spmd(_NC_CACHE["nc"], maps, core_ids=list(range(8)))
    out = np.zeros((B, SEQ, D), np.float32)
    for i, (b, ctx, own) in enumerate(cores):
        out[b, own] = res.results[i]["out"]
    return out
```
